# Optimizing a Trainium2 kernel written in Bass

```python
import math
import jax, jax.numpy as jnp
from jax import lax
import numpy as np

D_MODEL = 1024
BATCH = 2
SEQ = 16384
DEPTH = 2

PLE_DIM = 256
HEAD_DIM = 64
DIFF_HEADS = 4
DSA_HEADS = 8
IDX_HEADS = 8
IDX_DIM = 64
TOPK_MAX = 256
N_GROUPS = 4
EXPERTS_PER_GROUP = 8
TOP_K_IN_GROUP = 2
D_EXPERT = 256
ROPE_THETA = 500000.0
ROT_DIM = HEAD_DIM // 4
Q_BLOCK = 128
LN_EPS = 1e-5
NEG_INF = -1e30
ALPHA = (2 * DEPTH) ** 0.25
BETA = (8 * DEPTH) ** -0.25
DIFF_WIDTH = DIFF_HEADS * 2 * HEAD_DIM
DSA_WIDTH = DSA_HEADS * HEAD_DIM
SPLIT_SIZES = (DIFF_WIDTH, DIFF_WIDTH, DIFF_WIDTH, DSA_WIDTH, DSA_WIDTH, DSA_WIDTH,
               IDX_HEADS * IDX_DIM, IDX_DIM, IDX_HEADS, D_MODEL, D_MODEL)
W_IN_COLS = sum(SPLIT_SIZES)

kernel_name = 'hybrid_diffattn_dsa_hiermoe_deepnorm'


def layer_norm(x, g, b):
    xf = x.astype(jnp.float32)
    mu = jnp.mean(xf, axis=-1, keepdims=True)
    var = jnp.mean(jnp.square(xf - mu), axis=-1, keepdims=True)
    y = (xf - mu) * lax.rsqrt(var + LN_EPS)
    return (y * g.astype(jnp.float32) + b.astype(jnp.float32)).astype(x.dtype)


def rms_norm(x, g):
    xf = x.astype(jnp.float32)
    y = xf * lax.rsqrt(jnp.mean(jnp.square(xf), axis=-1, keepdims=True) + LN_EPS)
    return (y * g.astype(jnp.float32)).astype(x.dtype)


def rope_tables(positions):
    inv_freq = 1.0 / (ROPE_THETA ** (jnp.arange(0, ROT_DIM, 2, dtype=jnp.float32) / ROT_DIM))
    ang = positions.astype(jnp.float32)[..., None] * inv_freq
    return jnp.cos(ang), jnp.sin(ang)


def partial_rope(t, cos, sin):
    half = ROT_DIM // 2
    shp = cos.shape[:2] + (1,) * (t.ndim - 3) + (half,)
    c = cos.reshape(shp).astype(t.dtype)
    s = sin.reshape(shp).astype(t.dtype)
    x1 = t[..., :half]
    x2 = t[..., half:ROT_DIM]
    return jnp.concatenate([x1 * c - x2 * s, x2 * c + x1 * s, t[..., ROT_DIM:]], axis=-1)


def diff_attention(q, k, v, lam):
    B, S, H, _, dk = q.shape
    nb = S // Q_BLOCK
    scale = dk ** -0.5
    qb = q.reshape(B, nb, Q_BLOCK, H, 2, dk).swapaxes(0, 1)
    key_pos = jnp.arange(S)

    def one_block(args):
        q_i, b_i = args
        q_pos = b_i * Q_BLOCK + jnp.arange(Q_BLOCK)
        s = jnp.einsum('bqhcd,bkhcd->bhcqk', q_i, k).astype(jnp.float32) * scale
        s = jnp.where(key_pos[None, :] <= q_pos[:, None], s, NEG_INF)
        pr = jax.nn.softmax(s, axis=-1)
        a = pr[:, :, 0] - lam * pr[:, :, 1]
        return jnp.einsum('bhqk,bkhe->bqhe', a.astype(v.dtype), v)

    o = lax.map(one_block, (qb, jnp.arange(nb)))
    return o.swapaxes(0, 1).reshape(B, S, H, v.shape[-1])


def dsa_attention(q, k, v, q_idx, k_idx, w_idx, topk):
    B, S, H, dh = q.shape
    nb = S // Q_BLOCK
    scale = dh ** -0.5
    to_blocks = lambda t: t.reshape((B, nb, Q_BLOCK) + t.shape[2:]).swapaxes(0, 1)
    key_pos = jnp.arange(S)

    def one_block(args):
        q_b, qi_b, wi_b, b_i = args
        q_pos = b_i * Q_BLOCK + jnp.arange(Q_BLOCK)
        dots = jnp.einsum('bqhd,bsd->bqhs', qi_b, k_idx).astype(jnp.float32) * (IDX_DIM ** -0.5)
        score = jnp.einsum('bqhs,bqh->bqs', jax.nn.relu(dots), wi_b.astype(jnp.float32))
        score = jnp.where(key_pos[None, None, :] <= q_pos[None, :, None], score, NEG_INF)
        _, sel = lax.top_k(score, topk)
        valid = sel <= q_pos[None, :, None]
        kg = jax.vmap(lambda kb, ib: kb[ib])(k, sel)
        vg = jax.vmap(lambda vb, ib: vb[ib])(v, sel)
        s = jnp.einsum('bqhd,bqkhd->bhqk', q_b, kg).astype(jnp.float32) * scale
        s = jnp.where(valid[:, None], s, NEG_INF)
        pr = jax.nn.softmax(s, axis=-1)
        return jnp.einsum('bhqk,bqkhd->bqhd', pr.astype(v.dtype), vg)

    o = lax.map(one_block, (to_blocks(q), to_blocks(q_idx), to_blocks(w_idx), jnp.arange(nb)))
    return o.swapaxes(0, 1).reshape(B, S, H * dh)


def token_mixers(x, cos, sin, w_in, lam_params, subln_g, w_bd, w_bs, w_o, lam_init):
    B, S, _ = x.shape
    h = x @ w_in
    points = [int(c) for c in np.cumsum(SPLIT_SIZES)[:-1]]
    dq, dkk, dv, sq, sk, sv, iq, ik, iw, ga, gb = jnp.split(h, points, axis=-1)

    dq = partial_rope(dq.reshape(B, S, DIFF_HEADS, 2, HEAD_DIM), cos, sin)
    dkk = partial_rope(dkk.reshape(B, S, DIFF_HEADS, 2, HEAD_DIM), cos, sin)
    dv = dv.reshape(B, S, DIFF_HEADS, 2 * HEAD_DIM)
    lp = lam_params.astype(jnp.float32)
    lam = jnp.exp(jnp.sum(lp[0] * lp[1])) - jnp.exp(jnp.sum(lp[2] * lp[3])) + lam_init
    ya = diff_attention(dq, dkk, dv, lam)
    ya = (rms_norm(ya, subln_g) * (1.0 - lam_init)).reshape(B, S, DIFF_WIDTH)

    sq = partial_rope(sq.reshape(B, S, DSA_HEADS, HEAD_DIM), cos, sin)
    sk = partial_rope(sk.reshape(B, S, DSA_HEADS, HEAD_DIM), cos, sin)
    sv = sv.reshape(B, S, DSA_HEADS, HEAD_DIM)
    iq = partial_rope(iq.reshape(B, S, IDX_HEADS, IDX_DIM), cos, sin)
    ik = partial_rope(ik, cos, sin)
    iw = iw * (IDX_HEADS ** -0.5)
    topk = min(TOPK_MAX, S // 4)
    yb = dsa_attention(sq, sk, sv, iq, ik, iw, topk)

    merged = jax.nn.sigmoid(ga) * (ya @ w_bd) + jax.nn.sigmoid(gb) * (yb @ w_bs)
    return merged @ w_o


def hierarchical_moe(x, w_rg, b_rg, w_re, b_re, w_g, w_u, w_d):
    B, S, D = x.shape
    xt = x.reshape(-1, D)
    n = xt.shape[0]
    g_prob = jax.nn.softmax((xt @ w_rg).astype(jnp.float32) + b_rg.astype(jnp.float32), axis=-1)
    g_val, g_idx = lax.top_k(g_prob, 1)
    e_logit = ((xt @ w_re).astype(jnp.float32) + b_re.astype(jnp.float32)).reshape(n, N_GROUPS, EXPERTS_PER_GROUP)
    e_logit = jnp.take_along_axis(e_logit, g_idx[:, :, None], axis=1)[:, 0]
    e_prob = jax.nn.softmax(e_logit, axis=-1)
    e_val, e_idx = lax.top_k(e_prob, TOP_K_IN_GROUP)
    e_val = e_val / jnp.sum(e_val, axis=-1, keepdims=True)
    within = jnp.sum(jax.nn.one_hot(e_idx, EXPERTS_PER_GROUP, dtype=jnp.float32) * e_val[..., None], axis=1)
    comb = jax.nn.one_hot(g_idx[:, 0], N_GROUPS, dtype=jnp.float32)[:, :, None] * (g_val * within)[:, None, :]
    y = jnp.zeros_like(xt)
    for g in range(N_GROUPS):
        hg = jax.nn.silu(jnp.einsum('nd,edf->nef', xt, w_g[g])) * jnp.einsum('nd,edf->nef', xt, w_u[g])
        y = y + jnp.einsum('nef,efd->nd', hg * comb[:, g, :, None].astype(hg.dtype), w_d[g])
    return y.reshape(B, S, D)


def setup_inputs(seed: int = 0) -> dict:
    key = jax.random.key(seed)
    ks = jax.random.split(key, 24)
    L, D = DEPTH, D_MODEL
    G, E, F = N_GROUPS, EXPERTS_PER_GROUP, D_EXPERT
    nrm = lambda k, shape, scale: jax.random.normal(k, shape, jnp.float32) * scale
    return {
        'x': nrm(ks[0], (BATCH, SEQ, D), 1.0),
        'p': nrm(ks[1], (L, BATCH, SEQ, PLE_DIM), 1.0),
        'positions': jnp.broadcast_to(jnp.arange(SEQ, dtype=jnp.int32), (BATCH, SEQ)),
        'w_in': nrm(ks[2], (L, D, W_IN_COLS), D ** -0.5),
        'diff_lambda': nrm(ks[3], (L, 4, HEAD_DIM), 0.1),
        'diff_subln_g': 1.0 + nrm(ks[4], (L, 2 * HEAD_DIM), 0.01),
        'w_branch_diff': nrm(ks[5], (L, DIFF_WIDTH, D), DIFF_WIDTH ** -0.5),
        'w_branch_dsa': nrm(ks[6], (L, DSA_WIDTH, D), DSA_WIDTH ** -0.5),
        'w_out': nrm(ks[7], (L, D, D), BETA * D ** -0.5),
        'ln1_g': 1.0 + nrm(ks[8], (L, D), 0.01),
        'ln1_b': nrm(ks[9], (L, D), 0.01),
        'w_route_group': nrm(ks[10], (L, D, G), D ** -0.5),
        'b_route_group': nrm(ks[11], (L, G), 0.01),
        'w_route_expert': nrm(ks[12], (L, D, G * E), D ** -0.5),
        'b_route_expert': nrm(ks[13], (L, G * E), 0.01),
        'w_exp_gate': nrm(ks[14], (L, G, E, D, F), D ** -0.5),
        'w_exp_up': nrm(ks[15], (L, G, E, D, F), D ** -0.5),
        'w_exp_down': nrm(ks[16], (L, G, E, F, D), BETA * F ** -0.5),
        'w_ple': nrm(ks[17], (L, PLE_DIM, D), BETA * PLE_DIM ** -0.5),
        'w_ple_gate': nrm(ks[18], (L, D, D), D ** -0.5),
        'ln2_g': 1.0 + nrm(ks[19], (L, D), 0.01),
        'ln2_b': nrm(ks[20], (L, D), 0.01),
    }


def reference(x, p, positions, w_in, diff_lambda, diff_subln_g, w_branch_diff, w_branch_dsa,
              w_out, ln1_g, ln1_b, w_route_group, b_route_group, w_route_expert, b_route_expert,
              w_exp_gate, w_exp_up, w_exp_down, w_ple, w_ple_gate, ln2_g, ln2_b):
    cos, sin = rope_tables(positions)
    for i in range(DEPTH):
        lam_init = 0.8 - 0.6 * math.exp(-0.3 * i)
        mix = token_mixers(x, cos, sin, w_in[i], diff_lambda[i], diff_subln_g[i],
                           w_branch_diff[i], w_branch_dsa[i], w_out[i], lam_init)
        x = layer_norm(ALPHA * x + mix, ln1_g[i], ln1_b[i])
        ffn = hierarchical_moe(x, w_route_group[i], b_route_group[i], w_route_expert[i],
                               b_route_expert[i], w_exp_gate[i], w_exp_up[i], w_exp_down[i])
        ple = jax.nn.sigmoid(x @ w_ple_gate[i]) * (p[i] @ w_ple[i])
        x = layer_norm(ALPHA * x + ffn + ple, ln2_g[i], ln2_b[i])
    return x
```

```python
import numpy as np
import math
from contextlib import ExitStack
import concourse.bass as bass
import concourse.mybir as mybir
from concourse.bass_utils import run_bass_kernel_spmd

F32 = mybir.dt.float32
BF16 = mybir.dt.bfloat16
I32 = mybir.dt.int32
AF = mybir.ActivationFunctionType
ALU = mybir.AluOpType
AX = mybir.AxisListType

D = 1024
KC = 8
DEPTH = 2
WCOLS = 5704
ALPHA = (2 * DEPTH) ** 0.25
LN_EPS = 1e-5
TOPK = 256
NEGM = -30000.0


class Sem:
    def __init__(self, h, idx):
        self.h = h
        self.idx = idx
        self.n = 0


class Buf:
    def __init__(self, t, dram=False, name=""):
        self.t = t
        self.dram = dram
        self.w = {}
        self.r = {}
        self.name = name

    def __getitem__(self, k):
        return self.t[k]


class Eng:
    def __init__(self, name, h, sem):
        self.name = name
        self.h = h
        self.sem = sem
        self.waited = {}

    def wait(self, sem, val):
        if self.waited.get(sem.idx, 0) >= val:
            return
        self.h.wait_ge(sem.h, val)
        self.waited[sem.idx] = val


class K:
    def __init__(self, nc, es):
        self.nc = nc
        self.es = es
        self.nsem = 0
        self.sems = {}
        self.eng = {}
        for name, h in (("pe", nc.tensor), ("act", nc.scalar), ("dve", nc.vector),
                        ("pool", nc.gpsimd), ("sp", nc.sync)):
            self.eng[name] = Eng(name, h, self.new_sem("e_" + name))
        self.free_dsems = [[], []]
        self.uid = 0

    def new_sem(self, name):
        h = self.es.enter_context(self.nc.semaphore(name))
        s = Sem(h, self.nsem)
        self.nsem += 1
        self.sems[s.idx] = s
        return s

    def get_dsem(self, sw=False):
        fl = self.free_dsems[1 if sw else 0]
        if fl:
            return fl.pop()
        sm = self.new_sem(("w%d" if sw else "d%d") % self.nsem)
        sm.sw = sw
        return sm

    def put_dsem(self, s):
        self.free_dsems[1 if s.sw else 0].append(s)

    def _deps(self, r, w, e, is_dma):
        deps = {}

        def add(d, same_ok):
            for idx, val in d.items():
                if (not is_dma) and idx == e.sem.idx and not same_ok:
                    continue
                if deps.get(idx, 0) < val:
                    deps[idx] = val
        for b in r:
            add(b.w, e.name != "pe")
        for b in w:
            add(b.r, e.name != "pe")
            if not b.dram:
                add(b.w, e.name != "pe")
        return deps

    def op(self, en, fn, r=(), w=()):
        e = self.eng[en]
        deps = self._deps(r, w, e, False)
        for idx, val in deps.items():
            e.wait(self.sems[idx], val)
        ins = fn(e.h)
        e.sem.n += 1
        ins.then_inc(e.sem.h, 1)
        n = e.sem.n
        for b in r:
            b.r[e.sem.idx] = n
        for b in w:
            if b.dram:
                b.w[e.sem.idx] = n
            else:
                b.w = {e.sem.idx: n}
                b.r = {}
        return ins

    def dma(self, qn, out_ap, in_ap, dsem, r=(), w=(), **kw):
        e = self.eng[qn]
        assert dsem.sw == (qn == "pool"), (qn, dsem.sw)
        deps = self._deps(r, w, e, True)
        if dsem.n > 0:
            deps[dsem.idx] = max(deps.get(dsem.idx, 0), dsem.n)
        for idx, val in deps.items():
            e.wait(self.sems[idx], val)
        ins = e.h.dma_start(out=out_ap, in_=in_ap, **kw)
        dsem.n += 16
        ins.then_inc(dsem.h, 16)
        for b in r:
            b.r[dsem.idx] = dsem.n
        for b in w:
            if b.dram:
                b.w[dsem.idx] = dsem.n
            else:
                b.w = {dsem.idx: dsem.n}
                b.r = {}
        return ins

    def sb(self, es, name, shape, dt):
        self.uid += 1
        t = es.enter_context(self.nc.sbuf_tensor("%s_%d" % (name, self.uid), list(shape), dt))
        return Buf(t, name=name)

    def ps(self, es, name, shape, dt=F32):
        self.uid += 1
        t = es.enter_context(self.nc.psum_tensor("%s_%d" % (name, self.uid), list(shape), dt))
        return Buf(t, name=name)

    def dram(self, name, shape, dt):
        t = self.nc.dram_tensor(name, list(shape), dt, kind="Internal")
        return Buf(t.ap(), dram=True, name=name)

    def barrier(self):
        for e in self.eng.values():
            for idx, sm in self.sems.items():
                if sm.n > 0:
                    e.wait(sm, sm.n)

    def finish(self):
        e = self.eng["sp"]
        for idx, s in self.sems.items():
            if s.n > 0:
                e.wait(s, s.n)


class Rot:
    def __init__(self, bufs, sems=None):
        self.bufs = bufs
        self.sems = sems
        self.i = -1

    def next(self):
        self.i = (self.i + 1) % len(self.bufs)
        if self.sems is not None:
            return self.bufs[self.i], self.sems[self.i]
        return self.bufs[self.i]


def _tt(out, in0, in1, op):
    return lambda e: e.tensor_tensor(out=out, in0=in0, in1=in1, op=op)


def _ts(out, in0, s1, s2, op0, op1=None, accum_out=None):
    if op1 is None:
        return lambda e: e.tensor_scalar(out=out, in0=in0, scalar1=s1, scalar2=None, op0=op0)
    if accum_out is None:
        return lambda e: e.tensor_scalar(out=out, in0=in0, scalar1=s1, scalar2=s2, op0=op0, op1=op1)
    return lambda e: e.tensor_scalar(out=out, in0=in0, scalar1=s1, scalar2=s2, op0=op0, op1=op1, accum_out=accum_out)


def _stt(out, in0, scalar, in1, op0, op1):
    return lambda e: e.scalar_tensor_tensor(out=out, in0=in0, scalar=scalar, in1=in1, op0=op0, op1=op1)


def _act(out, in_, func, **kw):
    return lambda e: e.activation(out=out, in_=in_, func=func, **kw)


def _cp(out, in_):
    return lambda e: e.tensor_copy(out=out, in_=in_)


def _mm(out, lhsT, rhs, start, stop):
    return lambda e: e.matmul(out, lhsT=lhsT, rhs=rhs, start=start, stop=stop, skip_group_check=True)


def _tr(out, in_, ident):
    return lambda e: e.transpose(out=out, in_=in_, identity=ident)


class Prog:
    def __init__(self, S, layers, gather, debug=False, phases="XABCDEF"):
        self.debug = debug
        self.phases = phases
        self.declared = []
        self.abstop = 99
        self.fstop = 99
        self.S = S
        self.T = S // 4
        self.NT = self.T // 128
        self.NG = self.T // 512
        self.NSB = S // 512
        self.NTA = S // 128
        self.layers = layers
        self.gather = gather
        self.nc = bass.Bass("TRN2", target_bir_lowering=False)
        self.evac_i = 0

    def inp(self, name, shape, dt=F32):
        self.declared.append(name)
        return Buf(self.nc.dram_tensor(name, list(shape), dt, kind="ExternalInput").ap(), dram=True, name=name)

    def evac(self, out, in_, r, w):
        self.evac_i += 1
        if self.evac_i % 2:
            self.k.op("act", lambda e: e.copy(out=out, in_=in_), r=r, w=w)
        else:
            self.k.op("dve", _cp(out, in_), r=r, w=w)

    def build(self):
        nc = self.nc
        S, T, NT, NG, NSB, NTA = self.S, self.T, self.NT, self.NG, self.NSB, self.NTA
        L = DEPTH
        self.xall = self.inp("xall", [S, D])
        self.xq = self.inp("xq", [T, D])
        self.p = self.inp("p", [L, T, 256])
        self.posall = self.inp("posall", [128, NTA], I32)
        self.posq = self.inp("posq", [128, NT], I32)
        self.cmT_in = self.inp("cmT", [128, 16 * 512])
        self.cmQ_in = self.inp("cmQ", [128, 4 * 512])
        self.cmS_in = self.inp("cmS", [128, 8])
        self.w_in = self.inp("w_in", [L, D, WCOLS])
        self.diff_lambda = self.inp("diff_lambda", [L, 256])
        self.subln_g = self.inp("diff_subln_g", [L, 128])
        self.w_bd = self.inp("w_branch_diff", [L, 512, D])
        self.w_bs = self.inp("w_branch_dsa", [L, 512, D])
        self.w_o = self.inp("w_out", [L, D, D])
        self.ln1_g = self.inp("ln1_g", [L, D])
        self.ln1_b = self.inp("ln1_b", [L, D])
        self.w_rg = self.inp("w_route_group", [L, D, 4])
        self.b_rg = self.inp("b_route_group", [L, 4])
        self.w_re = self.inp("w_route_expert", [L, D, 32])
        self.b_re = self.inp("b_route_expert", [L, 32])
        if "X" in self.phases:
            self.w_eg = self.inp("w_exp_gate", [L, 32, D, 256])
            self.w_eu = self.inp("w_exp_up", [L, 32, D, 256])
            self.w_ed = self.inp("w_exp_down", [L, 32, 256, D])
        self.w_ple = self.inp("w_ple", [L, 256, D])
        self.w_pg = self.inp("w_ple_gate", [L, D, D])
        self.ln2_g = self.inp("ln2_g", [L, D])
        self.ln2_b = self.inp("ln2_b", [L, D])
        self.out = Buf(nc.dram_tensor("out", [T, D], F32, kind="ExternalOutput").ap(), dram=True, name="out")

        with ExitStack() as es:
            k = K(nc, es)
            self.k = k
            self.dKT = k.dram("dKT", [4, 128, S], BF16)
            self.sKT = k.dram("sKT", [4, 128, S], BF16)
            self.iKT = k.dram("iKT", [64, S], BF16)
            self.dV = k.dram("dV", [S, 512], BF16)
            self.sV = k.dram("sV", [S, 512], BF16)
            self.dQT = k.dram("dQT", [4, 128, T], BF16)
            self.sQT = k.dram("sQT", [4, 128, T], BF16)
            self.iQT = k.dram("iQT", [4, 128, T], BF16)
            self.sg = k.dram("sg", [T, 2048], F32)
            if self.debug:
                dbg = lambda n, sh, dt: Buf(nc.dram_tensor(n, list(sh), dt, kind="ExternalOutput").ap(), dram=True, name=n)
            else:
                dbg = k.dram
            self.ya = dbg("ya", [T, 512], BF16)
            self.yb = dbg("yb", [T, 512], BF16)
            self.x1 = dbg("x1", [T, D], F32)
            self.wg16 = [k.dram("wg16_%d" % l, [32, D, 256], BF16) for l in range(L)]
            self.wu16 = [k.dram("wu16_%d" % l, [32, D, 256], BF16) for l in range(L)]
            self.wd16 = [k.dram("wd16_%d" % l, [32, 256, D], BF16) for l in range(L)]
            if len(self.layers) > 1:
                self.xown = k.dram("xown", [T, D], F32)
                self.xall2 = k.dram("xall2", [S, D], F32)
            self.identf = k.sb(es, "identf", [128, 128], F32)
            self.identb = k.sb(es, "identb", [128, 128], BF16)
            self.tabA = k.sb(es, "tabA", [128, NTA, 16], F32)
            self.tabQ = k.sb(es, "tabQ", [128, NT, 16], F32)
            self.iwq = k.sb(es, "iwq", [128, NT, 8], F32)
            self.lam = k.sb(es, "lam", [128, 4], F32)
            self.gsc = k.sb(es, "gsc", [128, 128], F32)
            self.phase0()
            for l in self.layers:
                if "X" in self.phases:
                    self.cast_expert_weights(l)
            for li, l in enumerate(self.layers):
                if li == 0:
                    xall, xq = self.xall, self.xq
                else:
                    xall, xq = self.xall2, self.xown
                last = (li == len(self.layers) - 1)
                dst = self.out if last else self.xown
                self.layer_consts(l)
                if "A" in self.phases:
                    self.phaseAB(l, xall, True)
                if "B" in self.phases:
                    self.phaseAB(l, xq, False)
                if "C" in self.phases:
                    self.phaseC(l)
                if "D" in self.phases:
                    self.phaseD(l)
                if "E" in self.phases:
                    self.phaseE(l, xq)
                if "F" in self.phases:
                    self.phaseF(l, dst)
                if not last:
                    self.do_gather()
            k.finish()
        return nc

    def phase0(self):
        k = self.k
        with ExitStack() as es:
            idf, idb = self.identf, self.identb
            k.op("pool", lambda e: e.memset(idf[:], 0.0), w=[idf])
            k.op("pool", lambda e: e.affine_select(out=idf[:], in_=idf[:], pattern=[[-1, 128]],
                                                   compare_op=ALU.not_equal, fill=1.0, base=0,
                                                   channel_multiplier=1), r=[idf], w=[idf])
            k.op("dve", _cp(idb[:], idf[:]), r=[idf], w=[idb])
            theta = np.float32(500000.0)
            invf = (np.float32(1.0) / (theta ** (np.arange(0, 16, 2, dtype=np.float32) / np.float32(16)))).astype(np.float32)
            C1 = 6.28125
            C2 = float(2.0 * math.pi - C1)
            for tab, posd, n in ((self.tabA, self.posall, self.NTA), (self.tabQ, self.posq, self.NT)):
                with ExitStack() as es2:
                    pi = k.sb(es2, "pi", [128, n], I32)
                    pf = k.sb(es2, "pf", [128, n], F32)
                    ang = k.sb(es2, "ang", [128, n, 16], F32)
                    kk = k.sb(es2, "kk", [128, n, 16], F32)
                    ki = k.sb(es2, "ki", [128, n, 16], I32)
                    ds = k.get_dsem()
                    k.dma("sp", pi[:], posd.t[:, :], ds, r=[posd], w=[pi])
                    k.op("dve", _cp(pf[:], pi[:]), r=[pi], w=[pf])
                    for i in range(8):
                        k.op("dve", _ts(ang[:, :, 8 + i:9 + i], pf[:].unsqueeze(2), float(invf[i]), None, ALU.mult),
                             r=[pf], w=[ang])
                    k.op("dve", _ts(ang[:, :, 0:8], ang[:, :, 8:16], float(math.pi / 2), None, ALU.add), r=[ang], w=[ang])
                    k.op("dve", _ts(kk[:], ang[:], float(1.0 / (2 * math.pi)), None, ALU.mult), r=[ang], w=[kk])
                    k.op("dve", _cp(ki[:], kk[:]), r=[kk], w=[ki])
                    k.op("dve", _cp(kk[:], ki[:]), r=[ki], w=[kk])
                    k.op("dve", _stt(ang[:], kk[:], -C1, ang[:], ALU.mult, ALU.add), r=[kk, ang], w=[ang])
                    k.op("dve", _stt(ang[:], kk[:], -C2, ang[:], ALU.mult, ALU.add), r=[kk, ang], w=[ang])
                    k.op("dve", _ts(kk[:], ang[:], float(math.pi), float(-2 * math.pi), ALU.is_gt, ALU.mult), r=[ang], w=[kk])
                    k.op("dve", _tt(ang[:], ang[:], kk[:], ALU.add), r=[ang, kk], w=[ang])
                    k.op("dve", _ts(kk[:], ang[:], float(-math.pi), float(2 * math.pi), ALU.is_lt, ALU.mult), r=[ang], w=[kk])
                    k.op("dve", _tt(ang[:], ang[:], kk[:], ALU.add), r=[ang, kk], w=[ang])
                    k.op("dve", _ts(ang[:], ang[:], float(math.pi), float(-math.pi), ALU.min, ALU.max), r=[ang], w=[ang])
                    k.op("act", _act(tab[:], ang[:], AF.Sin), r=[ang], w=[tab])
                    k.put_dsem(ds)
                    k.barrier()

    def cast_expert_weights(self, l):
        k = self.k
        sems = [k.get_dsem(sw=True) for _ in range(6)]
        i = 0
        for E in range(32):
            for src, dst in ((self.w_eg, self.wg16[l]), (self.w_eu, self.wu16[l]), (self.w_ed, self.wd16[l])):
                k.dma("pool", dst.t[E], src.t[l, E], sems[i % 6], r=[src], w=[dst])
                i += 1
        for s in sems:
            k.put_dsem(s)

    def layer_consts(self, l):
        k = self.k
        lam_init = 0.8 - 0.6 * math.exp(-0.3 * l)
        with ExitStack() as es:
            lp = k.sb(es, "lp", [128, 256], F32)
            tmp = k.sb(es, "lptmp", [128, 128], F32)
            ss = k.sb(es, "lpss", [128, 2], F32)
            ds = k.get_dsem()
            k.dma("sp", lp[:], self.diff_lambda.t[l:l + 1, :].partition_broadcast(128), ds, r=[self.diff_lambda], w=[lp])
            k.op("dve", _tt(tmp[:, 0:64], lp[:, 0:64], lp[:, 64:128], ALU.mult), r=[lp], w=[tmp])
            k.op("dve", _tt(tmp[:, 64:128], lp[:, 128:192], lp[:, 192:256], ALU.mult), r=[lp], w=[tmp])
            k.op("dve", lambda e: e.tensor_reduce(out=ss[:, 0:2], in_=tmp[:].rearrange("p (a b) -> p a b", a=2),
                                                  axis=AX.X, op=ALU.add), r=[tmp], w=[ss])
            k.op("act", _act(ss[:], ss[:], AF.Exp), r=[ss], w=[ss])
            lam = self.lam
            k.op("dve", _tt(lam[:, 0:1], ss[:, 0:1], ss[:, 1:2], ALU.subtract), r=[ss], w=[lam])
            k.op("dve", _ts(lam[:, 0:1], lam[:, 0:1], float(lam_init), None, ALU.add), r=[lam], w=[lam])
            k.op("dve", _ts(lam[:, 1:2], lam[:, 0:1], -1.0, None, ALU.mult), r=[lam], w=[lam])
            k.dma("sp", self.gsc[:], self.subln_g.t[l:l + 1, :].partition_broadcast(128), ds, r=[self.subln_g], w=[self.gsc])
            k.op("dve", _ts(self.gsc[:], self.gsc[:], float(1.0 - lam_init), None, ALU.mult), r=[self.gsc], w=[self.gsc])
            k.put_dsem(ds)
            k.barrier()

    def xall_rows(self, g):
        return (g % 4) * self.T + (g // 4) * 512

    def rope(self, es_tmp, ps, w, dst, tab_c, tab_s, rbufs, wbuf):
        k = self.k
        nh = w // 64
        psv = ps[:, 0:w].rearrange("p (h d) -> p h d", d=64)
        dv = dst.rearrange("p (h d) -> p h d", d=64)
        c = tab_c.unsqueeze(1).to_broadcast([128, nh, 8])
        s = tab_s.unsqueeze(1).to_broadcast([128, nh, 8])
        k.op("act", lambda e: e.copy(out=dv[:, :, 16:64], in_=psv[:, :, 16:64]), r=rbufs, w=[wbuf])
        if self.abstop < 2.35:
            return
        r16 = es_tmp.next()
        k.op("act", lambda e: e.copy(out=r16[:, 0:nh, :], in_=psv[:, :, 0:16]), r=rbufs, w=[r16])
        t1, t2 = es_tmp.next(), es_tmp.next()
        k.op("dve", _tt(t1[:, 0:nh, 0:8], r16[:, 0:nh, 0:8], c, ALU.mult), r=[r16] + rbufs, w=[t1])
        k.op("dve", _tt(t2[:, 0:nh, 0:8], r16[:, 0:nh, 8:16], s, ALU.mult), r=[r16] + rbufs, w=[t2])
        k.op("dve", _tt(dv[:, :, 0:8], t1[:, 0:nh, 0:8], t2[:, 0:nh, 0:8], ALU.subtract), r=[t1, t2], w=[wbuf])
        k.op("dve", _tt(t1[:, 0:nh, 8:16], r16[:, 0:nh, 8:16], c, ALU.mult), r=[r16] + rbufs, w=[t1])
        k.op("dve", _tt(t2[:, 0:nh, 8:16], r16[:, 0:nh, 0:8], s, ALU.mult), r=[r16] + rbufs, w=[t2])
        k.op("dve", _tt(dv[:, :, 8:16], t1[:, 0:nh, 8:16], t2[:, 0:nh, 8:16], ALU.add), r=[t1, t2], w=[wbuf])

    def phaseAB(self, l, xsrc, isA):
        k = self.k
        S, T = self.S, self.T
        with ExitStack() as es:
            if isA:
                segs = [(512, 1536, 0), (2048, 3072, 1024), (3584, 3648, 2048)]
                wtot = 2112
                chunks = [("k", 0, 512, 0), ("v", 512, 512, 0), ("k", 1024, 512, 512), ("v", 1536, 512, 512),
                          ("k", 2048, 64, 1024)]
                kw = 1088
                nsb = self.NSB
                tab = self.tabA
            else:
                segs = [(0, 512, 0), (1536, 2048, 512), (3072, 3584, 1024), (3648, 5704, 1536)]
                wtot = 3592
                chunks = [("k", 0, 512, 0), ("k", 512, 512, 512), ("k", 1024, 512, 1024), ("w", 1536, 8, 0)] + \
                         [("g", 1544 + i * 512, 512, i * 512) for i in range(4)]
                kw = 1536
                nsb = self.NG
                tab = self.tabQ
            nkt = (kw + 127) // 128
            W = k.sb(es, "W", [128, KC, wtot], BF16)
            wsem = k.get_dsem(sw=True)
            for (c0, c1, o) in segs:
                k.dma("pool", W[:, :, o:o + (c1 - c0)],
                      self.w_in.t[l, :, c0:c1].rearrange("(kc p) c -> p kc c", p=128), wsem, r=[self.w_in], w=[W])
            xsR = Rot([k.sb(es, "xs", [128, 4, D], BF16) for _ in range(2)], [k.get_dsem(sw=True) for _ in range(2)])
            xTR = Rot([k.sb(es, "xT", [128, KC, 512], BF16) for _ in range(2)])
            kroR = Rot([k.sb(es, "kro", [128, 4, kw], BF16) for _ in range(2)])
            kstR = Rot([k.sb(es, "kst", [128, nkt, 512], BF16) for _ in range(2)])
            ksem = [[k.get_dsem() for _ in range(3)] for _ in range(2)]
            if isA:
                voR = Rot([k.sb(es, "vo", [128, 4, 1024], BF16) for _ in range(2)])
                vsem = [[k.get_dsem() for _ in range(2)] for _ in range(2)]
            else:
                sgR = Rot([k.sb(es, "sgt", [128, 2048], F32) for _ in range(2)], [k.get_dsem() for _ in range(2)])
            tmpR = Rot([k.sb(es, "rt", [128, 8, 16], F32) for _ in range(9)])
            tpR = Rot([k.ps(es, "tp", [128, 1024], BF16) for _ in range(2)])
            psR = Rot([k.ps(es, "ps", [128, 512], F32) for _ in range(3)])
            ktR = Rot([k.ps(es, "kt", [128, 1024], BF16) for _ in range(2)])
            idb = self.identb
            for g in range(nsb):
                r0 = self.xall_rows(g) if isA else g * 512
                xs, xsem = xsR.next()
                k.dma("pool", xs[:], xsrc.t[r0:r0 + 512, :].rearrange("(t p) d -> p t d", p=128), xsem, r=[xsrc], w=[xs])
                xT = xTR.next()
                if self.abstop < 2:
                    continue
                for kc in range(KC):
                    tp = tpR.next()
                    for t in range(4):
                        k.op("pe", _tr(tp[:, t * 128:(t + 1) * 128], xs[:, t, kc * 128:(kc + 1) * 128], idb[:]),
                             r=[xs, idb], w=[tp])
                    self.evac(xT[:, kc, :], tp[:, 0:512], [tp], [xT])
                kro = kroR.next()
                ki = kroR.i
                if self.abstop < 2.05:
                    continue
                if isA:
                    vo = voR.next()
                else:
                    pass
                for t in range(4):
                    n = g * 4 + t
                    if not isA:
                        sgt, sgsem = sgR.next()
                    for (kind, wo, w, dst) in chunks:
                        ps = psR.next()
                        for kc in range(KC):
                            k.op("pe", _mm(ps[:, 0:w], xT[:, kc, t * 128:(t + 1) * 128], W[:, kc, wo:wo + w],
                                           kc == 0, kc == KC - 1), r=[xT, W], w=[ps])
                        if self.abstop < 2.15:
                            continue
                        if kind == "k":
                            if self.abstop < 2.25:
                                continue
                            self.rope(tmpR, ps, w, kro[:, t, dst:dst + w], tab[:, n, 0:8], tab[:, n, 8:16],
                                      [ps, tab], kro)
                        elif kind == "v":
                            self.evac(vo[:, t, dst:dst + w], ps[:, 0:w], [ps], [vo])
                        elif kind == "w":
                            k.op("dve", _ts(self.iwq[:, n, :], ps[:, 0:8], float(8 ** -0.5 * 0.125), None, ALU.mult),
                                 r=[ps], w=[self.iwq])
                        else:
                            k.op("act", _act(sgt[:, dst:dst + w], ps[:, 0:w], AF.Sigmoid), r=[ps], w=[sgt])
                    if not isA:
                        k.dma("sp", self.sg.t[n * 128:(n + 1) * 128, :], sgt[:], sgsem, r=[sgt], w=[self.sg])
                kst = kstR.next()
                if self.abstop < 4:
                    continue
                for hh in range(nkt):
                    cw = min(128, kw - hh * 128)
                    kt = ktR.next()
                    for t in range(4):
                        k.op("pe", _tr(kt[0:cw, t * 128:(t + 1) * 128], kro[:, t, hh * 128:hh * 128 + cw], idb[:]),
                             r=[kro, idb], w=[kt])
                    self.evac(kst[0:cw, hh, :], kt[0:cw, 0:512], [kt], [kst])
                sl = slice(g * 512, (g + 1) * 512)
                if self.abstop < 5:
                    continue
                if isA:
                    k.dma("sp", self.dKT.t[:, :, sl].rearrange("h p s -> p h s"), kst[:, 0:4, :], ksem[ki][0], r=[kst], w=[self.dKT])
                    k.dma("sp", self.sKT.t[:, :, sl].rearrange("h p s -> p h s"), kst[:, 4:8, :], ksem[ki][1], r=[kst], w=[self.sKT])
                    k.dma("sp", self.iKT.t[:, sl], kst[0:64, 8, :], ksem[ki][2], r=[kst], w=[self.iKT])
                    k.dma("sp", self.dV.t[sl, :].rearrange("(t p) c -> p t c", p=128), vo[:, :, 0:512], vsem[ki][0], r=[vo], w=[self.dV])
                    k.dma("sp", self.sV.t[sl, :].rearrange("(t p) c -> p t c", p=128), vo[:, :, 512:1024], vsem[ki][1], r=[vo], w=[self.sV])
                else:
                    k.dma("sp", self.dQT.t[:, :, sl].rearrange("h p s -> p h s"), kst[:, 0:4, :], ksem[ki][0], r=[kst], w=[self.dQT])
                    k.dma("sp", self.sQT.t[:, :, sl].rearrange("h p s -> p h s"), kst[:, 4:8, :], ksem[ki][1], r=[kst], w=[self.sQT])
                    k.dma("sp", self.iQT.t[:, :, sl].rearrange("h p s -> p h s"), kst[:, 8:12, :], ksem[ki][2], r=[kst], w=[self.iQT])
            for s_ in [wsem] + xsR.sems + sum(ksem, []) + (sum(vsem, []) if isA else sgR.sems):
                k.put_dsem(s_)
            k.barrier()

    def phaseC(self, l):
        k = self.k
        S, T, NG = self.S, self.T, self.NG
        NKB = S // 128
        lam, gsc = self.lam, self.gsc
        with ExitStack() as es:
            cmT = k.sb(es, "cmT", [128, 16, 512], BF16)
            sems = [k.get_dsem(sw=True)] + [k.get_dsem() for _ in range(5)]
            k.dma("pool", cmT[:], self.cmT_in.t[:, :].rearrange("p (a b) -> p a b", a=16), sems[0], r=[self.cmT_in], w=[cmT])
            KTh = k.sb(es, "KTh", [128, S], BF16)
            Vh = k.sb(es, "Vh", [128, NKB, 130], BF16)
            QTh = k.sb(es, "QTh", [128, T], BF16)
            k.op("pool", lambda e: e.memset(Vh[:, :, 128:130], 1.0), w=[Vh])
            stR = [Rot([k.ps(es, "st", [128, 512]) for _ in range(2)]) for c in range(2)]
            oacc = [k.ps(es, "oacc", [128, 512]) for _ in range(4)]
            pTR = [Rot([k.sb(es, "pT", [128, 512], BF16) for _ in range(3)]) for c in range(2)]
            yaR = Rot([k.sb(es, "yast", [128, 4, 128], BF16) for _ in range(2)], sems[4:6])
            rlR = Rot([k.sb(es, "rl", [128, 2], F32) for _ in range(2)])
            d1R = Rot([k.sb(es, "d1", [128, 128], F32) for _ in range(2)])
            ddR = Rot([k.sb(es, "dd", [128, 128], F32) for _ in range(2)])
            ssR = Rot([k.sb(es, "ss", [128, 2], F32) for _ in range(2)])
            junk = k.sb(es, "junk", [128, 128], F32)
            for h in range(4):
                k.dma("sp", KTh[:], self.dKT.t[h], sems[1], r=[self.dKT], w=[KTh])
                k.dma("sp", Vh[:, :, 0:128], self.dV.t[:, h * 128:(h + 1) * 128].rearrange("(n p) c -> p n c", p=128),
                      sems[2], r=[self.dV], w=[Vh])
                k.dma("sp", QTh[:], self.dQT.t[h], sems[3], r=[self.dQT], w=[QTh])
                for j in range(NG):
                    nkb = 16 * (j + 1)
                    prev = None
                    for n in range(nkb + 1):
                        cur = None
                        if n < nkb:
                            cur = []
                            for c in range(2):
                                st = stR[c].next()
                                k.op("pe", _mm(st[:, :], KTh[c * 64:(c + 1) * 64, n * 128:(n + 1) * 128],
                                               QTh[c * 64:(c + 1) * 64, j * 512:(j + 1) * 512], True, True),
                                     r=[KTh, QTh], w=[st])
                                pT = pTR[c].next()
                                k.op("act", _act(pT[:], st[:], AF.Exp, scale=0.125), r=[st], w=[pT])
                                if n >= 16 * j:
                                    k.op("pool", _tt(pT[:], pT[:], cmT[:, n - 16 * j, :], ALU.mult), r=[pT, cmT], w=[pT])
                                cur.append(pT)
                        if prev is not None:
                            m, pTs = prev
                            for qs in range(4):
                                for c in range(2):
                                    k.op("pe", _mm(oacc[qs][:, c * 256:c * 256 + 129], pTs[c][:, qs * 128:(qs + 1) * 128],
                                                   Vh[:, m, 0:129], m == 0 and c == 0, m == nkb - 1), r=[pTs[c], Vh], w=[oacc[qs]])
                        prev = (n, cur) if cur is not None else None
                    yast, ysem = yaR.next()
                    for qs in range(4):
                        oa = oacc[qs]
                        oav = oa[:, :].rearrange("p (c x) -> p c x", c=2)
                        rl = rlR.next()
                        k.op("dve", lambda e: e.reciprocal(out=rl[:, :].unsqueeze(2), in_=oav[:, :, 128:129]), r=[oa], w=[rl])
                        k.op("dve", _tt(rl[:, 1:2], rl[:, 1:2], lam[:, 1:2], ALU.mult), r=[rl, lam], w=[rl])
                        d1 = d1R.next()
                        k.op("dve", _ts(d1[:], oa[:, 0:128], rl[:, 0:1], None, ALU.mult), r=[oa, rl], w=[d1])
                        dd = ddR.next()
                        k.op("dve", _stt(dd[:], oa[:, 256:384], rl[:, 1:2], d1[:], ALU.mult, ALU.add), r=[oa, rl, d1], w=[dd])
                        ss = ssR.next()
                        k.op("dve", lambda e: e.memset(ss[:], 0.0), w=[ss])
                        k.op("act", _act(junk[:], dd[:], AF.Square, accum_out=ss[:, 0:1]), r=[dd, ss], w=[junk, ss])
                        k.op("dve", _ts(ss[:, 1:2], ss[:, 0:1], 1.0 / 128, LN_EPS, ALU.mult, ALU.add), r=[ss], w=[ss])
                        k.op("act", _act(ss[:, 1:2], ss[:, 1:2], AF.Sqrt), r=[ss], w=[ss])
                        k.op("dve", lambda e: e.reciprocal(out=ss[:, 1:2], in_=ss[:, 1:2]), r=[ss], w=[ss])
                        k.op("dve", _stt(yast[:, qs, :], dd[:], ss[:, 1:2], gsc[:], ALU.mult, ALU.mult), r=[dd, ss, gsc], w=[yast])
                        if getattr(self, "dbgC", 0) == 1:
                            k.op("dve", _cp(yast[:, qs, :], d1[:]), r=[d1], w=[yast])
                        if getattr(self, "dbgC", 0) == 2:
                            k.op("dve", _cp(yast[:, qs, :], dd[:]), r=[dd], w=[yast])
                    k.dma("sp", self.ya.t[j * 512:(j + 1) * 512, h * 128:(h + 1) * 128].rearrange("(q p) c -> p q c", p=128),
                          yast[:], ysem, r=[yast], w=[self.ya])
            for s_ in sems:
                k.put_dsem(s_)
            k.barrier()

    def phaseD(self, l):
        k = self.k
        S, T, NT = self.S, self.T, self.NT
        NIT = 22
        idb = self.identb
        with ExitStack() as es:
            sems = [k.get_dsem(sw=(i_ == 1)) for i_ in range(12)]
            ikT2 = k.sb(es, "ikT2", [128, S], BF16)
            k.dma("sp", ikT2[0:64, :], self.iKT.t[:, :], sems[0], r=[self.iKT], w=[ikT2])
            k.dma("sp", ikT2[64:128, :], self.iKT.t[:, :], sems[0], r=[self.iKT], w=[ikT2])
            cmQ = k.sb(es, "cmQ", [128, 4, 512], BF16)
            cmS = k.sb(es, "cmS", [128, 8], F32)
            k.dma("pool", cmQ[:], self.cmQ_in.t[:, :].rearrange("p (a b) -> p a b", a=4), sems[1], r=[self.cmQ_in], w=[cmQ])
            k.dma("sp", cmS[:], self.cmS_in.t[:, :], sems[0], r=[self.cmS_in], w=[cmS])
            sc = k.sb(es, "sc", [128, S], F32)
            mbs = [k.sb(es, "mb", [128, S], BF16) for _ in range(2)]
            iqR = Rot([k.sb(es, "iqT", [128, 4, 128], BF16) for _ in range(2)], sems[2:4])
            sqR = Rot([k.sb(es, "sqT", [128, 4, 128], BF16) for _ in range(2)], sems[4:6])
            KcR = Rot([k.sb(es, "KTc", [128, 2048], BF16) for _ in range(2)], sems[6:8])
            VcR = Rot([k.sb(es, "Vc", [128, 16, 130], BF16) for _ in range(2)], sems[8:10])
            for vb in VcR.bufs:
                k.op("pool", lambda e: e.memset(vb[:, :, 0:1], 1.0), w=[vb])
                k.op("pool", lambda e: e.memset(vb[:, :, 129:130], 1.0), w=[vb])
            ybR = Rot([k.sb(es, "ybt", [128, 512], BF16) for _ in range(2)], sems[10:12])
            rlR = Rot([k.sb(es, "relu", [128, 512], F32) for _ in range(2)])
            pTR = Rot([k.sb(es, "pTd", [128, 512], BF16) for _ in range(3)])
            psI = Rot([k.ps(es, "psI", [128, 512]) for _ in range(3)])
            stR = Rot([k.ps(es, "stD", [128, 512]) for _ in range(3)])
            oaR = Rot([k.ps(es, "oaD", [128, 512]) for _ in range(2)])
            sm = k.sb(es, "sm", [128, 16], F32)
            r2R = Rot([k.sb(es, "r2", [128, 2], F32) for _ in range(2)])
            iwq = self.iwq

            def indexer_and_select(i, mb):
                j, qs = divmod(i, 4)
                NK = 2048 * (j + 1)
                iqT, isem = iqR.next()
                k.dma("sp", iqT[:], self.iQT.t[:, :, i * 128:(i + 1) * 128].rearrange("h p s -> p h s"), isem,
                      r=[self.iQT], w=[iqT])
                for ck in range(NK // 512):
                    cs = slice(ck * 512, (ck + 1) * 512)
                    for h in range(8):
                        hp, hh = divmod(h, 2)
                        ps = psI.next()
                        k.op("pe", _mm(ps[:, :], iqT[hh * 64:(hh + 1) * 64, hp, :], ikT2[hh * 64:(hh + 1) * 64, cs], True, True),
                             r=[iqT, ikT2], w=[ps])
                        rl = rlR.next()
                        k.op("act", _act(rl[:], ps[:], AF.Relu), r=[ps], w=[rl])
                        if h == 0:
                            k.op("dve", _ts(sc[:, cs], rl[:], iwq[:, i, 0:1], None, ALU.mult), r=[rl, iwq], w=[sc])
                        else:
                            k.op("dve", _stt(sc[:, cs], rl[:], iwq[:, i, h:h + 1], sc[:, cs], ALU.mult, ALU.add),
                                 r=[rl, iwq, sc], w=[sc])
                lo0, hi0, lo, w0, mid, cnt, tq, thr = [sm[:, a:a + 1] for a in range(8)]
                k.op("dve", lambda e: e.tensor_reduce(out=lo0, in_=sc[:, 0:NK], axis=AX.X, op=ALU.min), r=[sc], w=[sm])
                for sb in range(4):
                    blk = sc[:, NK - 2048 + sb * 512:NK - 2048 + (sb + 1) * 512]
                    k.op("dve", _stt(blk, cmQ[:, qs, :], cmS[:, 4 + sb:5 + sb], blk, ALU.mult, ALU.add), r=[sc, cmQ, cmS], w=[sc])
                    k.op("dve", _ts(blk, blk, cmS[:, sb:sb + 1], None, ALU.add), r=[sc, cmS], w=[sc])
                k.op("dve", lambda e: e.tensor_reduce(out=hi0, in_=sc[:, 0:NK], axis=AX.X, op=ALU.max), r=[sc], w=[sm])
                k.op("dve", _ts(lo, lo0, -1.0, None, ALU.add), r=[sm], w=[sm])
                k.op("dve", _stt(w0, hi0, 2.0, lo0, ALU.add, ALU.subtract), r=[sm], w=[sm])
                for it in range(NIT):
                    f = float(2.0 ** -(it + 1))
                    k.op("dve", _stt(mid, w0, f, lo, ALU.mult, ALU.add), r=[sm], w=[sm])
                    k.op("dve", _ts(mb[:, 0:NK], sc[:, 0:NK], mid, 0.0, ALU.is_ge, ALU.add, accum_out=cnt), r=[sc, sm], w=[mb, sm])
                    k.op("dve", _ts(tq, cnt, float(TOPK), f, ALU.is_gt, ALU.mult), r=[sm], w=[sm])
                    k.op("dve", _stt(lo, tq, w0, lo, ALU.mult, ALU.add), r=[sm], w=[sm])
                k.op("dve", _stt(thr, w0, float(2.0 ** -NIT), lo, ALU.mult, ALU.add), r=[sm], w=[sm])
                k.op("dve", _ts(mb[:, 0:NK], sc[:, 0:NK], thr, NEGM, ALU.is_lt, ALU.mult), r=[sc, sm], w=[mb])

            def dsa(i, mb):
                j, qs = divmod(i, 4)
                NK = 2048 * (j + 1)
                nkb = NK // 128
                sqT, ssem = sqR.next()
                k.dma("sp", sqT[:], self.sQT.t[:, :, i * 128:(i + 1) * 128].rearrange("h p s -> p h s"), ssem,
                      r=[self.sQT], w=[sqT])
                ybt, ysem = ybR.next()
                pend = []

                def flush():
                    while pend:
                        pend.pop(0)()
                for hp in range(4):
                    oacc = oaR.next()
                    for cki in range(NK // 2048):
                        KTc, ks = KcR.next()
                        Vc, vs = VcR.next()
                        k.dma("sp", KTc[:], self.sKT.t[hp, :, cki * 2048:(cki + 1) * 2048], ks, r=[self.sKT], w=[KTc])
                        k.dma("sp", Vc[:, :, 1:129],
                              self.sV.t[cki * 2048:(cki + 1) * 2048, hp * 128:(hp + 1) * 128].rearrange("(n p) c -> p n c", p=128),
                              vs, r=[self.sV], w=[Vc])
                        for kb2 in range(8):
                            st = stR.next()
                            for u in range(2):
                                kb = kb2 * 2 + u
                                n = cki * 16 + kb
                                for hh in range(2):
                                    slot = u * 2 + hh
                                    k.op("pe", _mm(st[:, slot * 128:(slot + 1) * 128], KTc[hh * 64:(hh + 1) * 64, kb * 128:(kb + 1) * 128],
                                                   sqT[hh * 64:(hh + 1) * 64, hp, :], True, False), r=[KTc, sqT], w=[st])
                                    k.op("pe", _mm(st[:, slot * 128:(slot + 1) * 128], mb[:, n * 128:(n + 1) * 128], idb[:], False, True),
                                         r=[mb, idb], w=[st])
                            pT = pTR.next()
                            k.op("act", _act(pT[:], st[:], AF.Exp, scale=0.125), r=[st], w=[pT])
                            flush()

                            def pv(pT=pT, Vc=Vc, oacc=oacc, kb2=kb2, cki=cki):
                                for u in range(2):
                                    kb = kb2 * 2 + u
                                    n = cki * 16 + kb
                                    for hh in range(2):
                                        slot = u * 2 + hh
                                        k.op("pe", _mm(oacc[:, hh * 65:(hh + 1) * 65], pT[:, slot * 128:(slot + 1) * 128],
                                                       Vc[:, kb, hh * 65:(hh + 1) * 65], n == 0 and hh == 0, n == nkb - 1),
                                             r=[pT, Vc], w=[oacc])
                            pend.append(pv)

                    def epi(oacc=oacc, hp=hp):
                        r2 = r2R.next()
                        k.op("dve", lambda e: e.reciprocal(out=r2[:, 0:1], in_=oacc[:, 0:1]), r=[oacc], w=[r2])
                        k.op("dve", lambda e: e.reciprocal(out=r2[:, 1:2], in_=oacc[:, 129:130]), r=[oacc], w=[r2])
                        k.op("dve", _ts(ybt[:, hp * 128:hp * 128 + 64], oacc[:, 1:65], r2[:, 0:1], None, ALU.mult), r=[oacc, r2], w=[ybt])
                        k.op("dve", _ts(ybt[:, hp * 128 + 64:hp * 128 + 128], oacc[:, 65:129], r2[:, 1:2], None, ALU.mult),
                             r=[oacc, r2], w=[ybt])
                    pend.append(epi)
                flush()
                k.dma("sp", self.yb.t[i * 128:(i + 1) * 128, :], ybt[:], ysem, r=[ybt], w=[self.yb])

            indexer_and_select(0, mbs[0])
            for i in range(NT):
                if i + 1 < NT:
                    indexer_and_select(i + 1, mbs[(i + 1) % 2])
                dsa(i, mbs[i % 2])
            for s_ in sems:
                k.put_dsem(s_)
            k.barrier()

    def layernorm(self, src, gB, bB, dst_ap, dstB, st6, mv):
        k = self.k
        for c in range(2):
            k.op("dve", lambda e: e.bn_stats(out=st6[:, c * 6:(c + 1) * 6], in_=src[:, c * 512:(c + 1) * 512]), r=[src], w=[st6])
        k.op("dve", lambda e: e.bn_aggr(out=mv[:, 0:2], in_=st6[:, 0:12]), r=[st6], w=[mv])
        k.op("dve", _ts(mv[:, 1:2], mv[:, 1:2], LN_EPS, None, ALU.add), r=[mv], w=[mv])
        k.op("act", _act(mv[:, 1:2], mv[:, 1:2], AF.Sqrt), r=[mv], w=[mv])
        k.op("dve", lambda e: e.reciprocal(out=mv[:, 1:2], in_=mv[:, 1:2]), r=[mv], w=[mv])
        k.op("dve", _ts(src[:, :], src[:, :], mv[:, 0:1], mv[:, 1:2], ALU.subtract, ALU.mult), r=[src, mv], w=[src])
        k.op("pool", _tt(src[:, :], src[:, :], gB[:, :], ALU.mult), r=[src, gB], w=[src])
        k.op("pool", _tt(dst_ap, src[:, :], bB[:, :], ALU.add), r=[src, bB], w=[dstB])

    def bcast_load(self, dstB, srcB, l, sem):
        self.k.dma("sp", dstB[:], srcB.t[l:l + 1, :].partition_broadcast(128), sem, r=[srcB], w=[dstB])

    def phaseE(self, l, xq):
        k = self.k
        NT = self.NT
        idb = self.identb
        with ExitStack() as es:
            sems = [k.get_dsem(sw=True)] + [k.get_dsem() for _ in range(11)]
            wbd = k.sb(es, "wbd", [128, 4, D], BF16)
            wbs = k.sb(es, "wbs", [128, 4, D], BF16)
            wo = k.sb(es, "wo", [128, 8, D], BF16)
            k.dma("pool", wbd[:], self.w_bd.t[l].rearrange("(kc p) c -> p kc c", p=128), sems[0], r=[self.w_bd], w=[wbd])
            k.dma("pool", wbs[:], self.w_bs.t[l].rearrange("(kc p) c -> p kc c", p=128), sems[0], r=[self.w_bs], w=[wbs])
            k.dma("pool", wo[:], self.w_o.t[l].rearrange("(kc p) c -> p kc c", p=128), sems[0], r=[self.w_o], w=[wo])
            g1 = k.sb(es, "g1", [128, D], F32)
            b1 = k.sb(es, "b1", [128, D], F32)
            self.bcast_load(g1, self.ln1_g, l, sems[1])
            self.bcast_load(b1, self.ln1_b, l, sems[1])
            yaR = Rot([k.sb(es, "yat", [128, 512], BF16) for _ in range(2)], sems[2:4])
            ybR = Rot([k.sb(es, "ybt", [128, 512], BF16) for _ in range(2)], sems[4:6])
            sgR = Rot([k.sb(es, "sgt", [128, 2048], F32) for _ in range(2)], sems[6:8])
            xR = Rot([k.sb(es, "xt", [128, D], F32) for _ in range(2)], sems[8:10])
            oR = Rot([k.sb(es, "x1t", [128, D], F32) for _ in range(2)], sems[10:12])
            yTR = Rot([k.sb(es, "yT", [128, 4, 128], BF16) for _ in range(4)])
            m1R = Rot([k.sb(es, "m1", [128, D], F32) for _ in range(2)])
            m2R = Rot([k.sb(es, "m2", [128, D], F32) for _ in range(2)])
            mgR = Rot([k.sb(es, "mg", [128, D], BF16) for _ in range(2)])
            mTR = Rot([k.sb(es, "mT", [128, 8, 128], BF16) for _ in range(2)])
            rR = Rot([k.sb(es, "r_", [128, D], F32) for _ in range(2)])
            st6 = k.sb(es, "st6", [128, 12], F32)
            mv = k.sb(es, "mv", [128, 2], F32)
            tpR = Rot([k.ps(es, "tpE", [128, 1024], BF16) for _ in range(2)])
            psR = Rot([k.ps(es, "psE", [128, 512]) for _ in range(6)])
            for i in range(NT):
                rows = slice(i * 128, (i + 1) * 128)
                yat, s_a = yaR.next()
                ybt, s_b = ybR.next()
                sgt, s_g = sgR.next()
                xt, s_x = xR.next()
                k.dma("sp", yat[:], self.ya.t[rows, :], s_a, r=[self.ya], w=[yat])
                k.dma("sp", ybt[:], self.yb.t[rows, :], s_b, r=[self.yb], w=[ybt])
                k.dma("sp", sgt[:], self.sg.t[rows, :], s_g, r=[self.sg], w=[sgt])
                k.dma("sp", xt[:], xq.t[rows, :], s_x, r=[xq], w=[xt])
                m12 = []
                for (yt, wb, goff, mR) in ((yat, wbd, 0, m1R), (ybt, wbs, 1024, m2R)):
                    tp = tpR.next()
                    for kc in range(4):
                        k.op("pe", _tr(tp[:, kc * 128:(kc + 1) * 128], yt[:, kc * 128:(kc + 1) * 128], idb[:]), r=[yt, idb], w=[tp])
                    yT = yTR.next()
                    self.evac(yT[:].rearrange("p a b -> p (a b)"), tp[:, 0:512], [tp], [yT])
                    m_ = mR.next()
                    for half in range(2):
                        ps = psR.next()
                        for kc in range(4):
                            k.op("pe", _mm(ps[:, :], yT[:, kc, :], wb[:, kc, half * 512:(half + 1) * 512], kc == 0, kc == 3),
                                 r=[yT, wb], w=[ps])
                        k.op("dve", _tt(m_[:, half * 512:(half + 1) * 512], ps[:, :], sgt[:, goff + half * 512:goff + (half + 1) * 512], ALU.mult),
                             r=[ps, sgt], w=[m_])
                    m12.append(m_)
                mg = mgR.next()
                k.op("pool", _tt(mg[:], m12[0][:], m12[1][:], ALU.add), r=m12, w=[mg])
                mT = mTR.next()
                for hf in range(2):
                    tp = tpR.next()
                    for kk in range(4):
                        kc = hf * 4 + kk
                        k.op("pe", _tr(tp[:, kk * 128:(kk + 1) * 128], mg[:, kc * 128:(kc + 1) * 128], idb[:]), r=[mg, idb], w=[tp])
                    self.evac(mT[:, hf * 4:(hf + 1) * 4, :].rearrange("p a b -> p (a b)"), tp[:, 0:512], [tp], [mT])
                r_ = rR.next()
                for half in range(2):
                    ps = psR.next()
                    for kc in range(8):
                        k.op("pe", _mm(ps[:, :], mT[:, kc, :], wo[:, kc, half * 512:(half + 1) * 512], kc == 0, kc == 7),
                             r=[mT, wo], w=[ps])
                    k.op("dve", _stt(r_[:, half * 512:(half + 1) * 512], xt[:, half * 512:(half + 1) * 512], float(ALPHA), ps[:, :],
                                     ALU.mult, ALU.add), r=[xt, ps], w=[r_])
                x1t, s_o = oR.next()
                self.layernorm(r_, g1, b1, x1t[:, :], x1t, st6, mv)
                k.dma("sp", self.x1.t[rows, :], x1t[:], s_o, r=[x1t], w=[self.x1])
            for s_ in sems:
                k.put_dsem(s_)
            k.barrier()

    def phaseF(self, l, dst):
        k = self.k
        NG = self.NG
        idb, idf = self.identb, self.identf
        with ExitStack() as es:
            sems = [k.get_dsem(sw=(i_ in (1, 17))) for i_ in range(18)]
            SK = getattr(self, "f0skip", "")
            wr32 = k.sb(es, "wr32", [128, 8, 36], F32)
            wrh = k.sb(es, "wrh", [128, 8, 64], BF16)
            wrl = k.sb(es, "wrl", [128, 8, 64], BF16)
            brt = k.sb(es, "brt", [128, 36], F32)
            if "w" not in SK:
                k.dma("sp", wr32[:, :, 0:4], self.w_rg.t[l].rearrange("(kc p) c -> p kc c", p=128), sems[0], r=[self.w_rg], w=[wr32])
                k.dma("sp", wr32[:, :, 4:36], self.w_re.t[l].rearrange("(kc p) c -> p kc c", p=128), sems[0], r=[self.w_re], w=[wr32])
                k.op("pool", lambda e: e.memset(wrh[:], 0.0), w=[wrh])
                k.op("pool", lambda e: e.memset(wrl[:], 0.0), w=[wrl])
                k.op("act", lambda e: e.copy(out=wrh[:, :, 0:36], in_=wr32[:]), r=[wr32], w=[wrh])
                k.op("dve", _tt(wrl[:, :, 0:36], wr32[:], wrh[:, :, 0:36], ALU.subtract), r=[wr32, wrh], w=[wrl])
                k.dma("sp", brt[:, 0:4], self.b_rg.t[l:l + 1, :].partition_broadcast(128), sems[0], r=[self.b_rg], w=[brt])
                k.dma("sp", brt[:, 4:36], self.b_re.t[l:l + 1, :].partition_broadcast(128), sems[0], r=[self.b_re], w=[brt])
            wpg = k.sb(es, "wpg", [128, 8, D], BF16)
            wple = k.sb(es, "wple", [128, 2, D], BF16)
            if "g" not in SK:
                k.dma("pool", wpg[:], self.w_pg.t[l].rearrange("(kc p) c -> p kc c", p=128), sems[1], r=[self.w_pg], w=[wpg])
                k.dma("pool", wple[:], self.w_ple.t[l].rearrange("(kc p) c -> p kc c", p=128), sems[1], r=[self.w_ple], w=[wple])
            g2 = k.sb(es, "g2", [128, D], F32)
            b2 = k.sb(es, "b2", [128, D], F32)
            if "b" not in SK:
                self.bcast_load(g2, self.ln2_g, l, sems[0])
                self.bcast_load(b2, self.ln2_b, l, sems[0])
            sel = k.sb(es, "sel", [32, 32, 128], BF16)
            if "s" not in SK:
                with ExitStack() as es0:
                    self_f = k.sb(es0, "self", [32, 32, 128], F32)
                    k.op("pool", lambda e: e.memset(self_f[:], 0.0), w=[self_f])
                    k.op("pool", lambda e: e.affine_select(out=self_f[:], in_=self_f[:], pattern=[[-1, 32], [0, 128]],
                                                           compare_op=ALU.not_equal, fill=1.0, base=0, channel_multiplier=1),
                         r=[self_f], w=[self_f])
                    k.op("pool", _cp(sel[:], self_f[:]), r=[self_f], w=[sel])
                    k.barrier()
            x1s = k.sb(es, "x1s", [128, 4, D], F32)
            pt16 = k.sb(es, "pt16", [128, 4, 256], BF16)
            x1h = k.sb(es, "x1h", [128, 4, D], BF16)
            x1l = k.sb(es, "x1l", [128, 4, D], BF16)
            x1T = k.sb(es, "x1T", [128, 8, 512], BF16)
            x1Tl = k.sb(es, "x1Tl", [128, 8, 512], BF16)
            combb = k.sb(es, "combb", [128, 32], BF16)
            combT = k.sb(es, "combT", [32, 512], BF16)
            yacc = k.sb(es, "yacc", [128, 4, D], F32)
            hT = [[k.sb(es, "hT", [128, 512], BF16) for _ in range(2)] for _ in range(8)]
            wdS = [k.sb(es, "wd", [128, 2, D], BF16) for _ in range(8)]
            wdsem = sems[2:10]
            wgR = Rot([k.sb(es, "wg", [128, 8, 256], BF16) for _ in range(2)], sems[10:12])
            wuR = Rot([k.sb(es, "wu", [128, 8, 256], BF16) for _ in range(2)], sems[12:14])
            cbR = Rot([k.sb(es, "cb", [128, 512], BF16) for _ in range(2)])
            sgR = Rot([k.sb(es, "sgl", [128, 512], BF16) for _ in range(2)])
            t1R = Rot([k.sb(es, "t1", [128, 512], BF16) for _ in range(2)])
            rt = k.sb(es, "rt", [128, 128], F32)
            comb = k.sb(es, "comb", [128, 32], F32)
            pTR = Rot([k.sb(es, "pT", [128, 2, 128], BF16) for _ in range(2)])
            sigR = Rot([k.sb(es, "sig", [128, D], F32) for _ in range(1)])
            rR = Rot([k.sb(es, "r2_", [128, D], F32) for _ in range(1)])
            oR = Rot([k.sb(es, "ot", [128, D], F32) for _ in range(2)], sems[14:16])
            st6 = k.sb(es, "st6", [128, 12], F32)
            mv = k.sb(es, "mv", [128, 2], F32)
            if getattr(self, "lgvar", 0) == 4:
                tpR = Rot([k.ps(es, "tpF", [128, 1024], BF16) for _ in range(2)])
                psR = Rot([k.ps(es, "psF", [128, 512]) for _ in range(6)])
            else:
                psR = Rot([k.ps(es, "psF", [128, 512]) for _ in range(6)])
                tpR = Rot([k.ps(es, "tpF", [128, 1024], BF16) for _ in range(2)])
            if getattr(self, "lgvar", 0) == 5:
                dm = psR.next()
                k.op("pe", _mm(dm[:, 0:128], idb[:], idb[:], True, True), r=[idb], w=[dm])
            for sg_i in range(NG):
                if self.fstop < 1:
                    continue
                rows = slice(sg_i * 512, (sg_i + 1) * 512)
                k.dma("sp", x1s[:], self.x1.t[rows, :].rearrange("(t p) d -> p t d", p=128), sems[16], r=[self.x1], w=[x1s])
                k.dma("pool", pt16[:], self.p.t[l, rows, :].rearrange("(t p) d -> p t d", p=128), sems[17], r=[self.p], w=[pt16])
                if self.fstop < 0.3:
                    continue
                k.op("act", lambda e: e.copy(out=x1h[:], in_=x1s[:]), r=[x1s], w=[x1h])
                k.op("dve", _tt(x1l[:], x1s[:], x1h[:], ALU.subtract), r=[x1s, x1h], w=[x1l])
                for kc in range(8):
                    if self.fstop < 0.5:
                        continue
                    tph = tpR.next()
                    for t in range(4):
                        k.op("pe", _tr(tph[:, t * 128:(t + 1) * 128], x1h[:, t, kc * 128:(kc + 1) * 128], idb[:]), r=[x1h, idb], w=[tph])
                    k.op("act", lambda e: e.copy(out=x1T[:, kc, :], in_=tph[:, 0:512]), r=[tph], w=[x1T])
                    tpl = tpR.next()
                    for t in range(4):
                        k.op("pe", _tr(tpl[:, t * 128:(t + 1) * 128], x1l[:, t, kc * 128:(kc + 1) * 128], idb[:]), r=[x1l, idb], w=[tpl])
                    k.op("dve", _cp(x1Tl[:, kc, :], tpl[:, 0:512]), r=[tpl], w=[x1Tl])
                if self.fstop < 1.2:
                    continue
                for t in range(4):
                    lg = psR.next()
                    ts_ = slice(t * 128, (t + 1) * 128)
                    for kc in range(8):
                        if getattr(self, "skiplg", 0):
                            continue
                        var = getattr(self, "lgvar", 0)
                        if var == 6:
                            k.op("pe", _mm(lg[:, 0:128], idb[:], idb[:], kc == 0, kc == 7), r=[idb], w=[lg])
                            continue
                        if var == 7:
                            if kc == 0:
                                k.op("pe", _mm(lg[:, 0:128], idb[:], idb[:], True, True), r=[idb], w=[lg])
                            continue
                        if var == 1:
                            k.op("pe", _mm(lg[:, 0:64], x1T[:, kc, ts_], wpg[:, kc, 0:64], kc == 0, kc == 7), r=[x1T, wpg], w=[lg])
                            continue
                        if var == 2:
                            k.op("pe", _mm(lg[:, 0:64], wpg[:, kc, 0:128], wrh[:, kc, :], kc == 0, kc == 7), r=[wpg, wrh], w=[lg])
                            continue
                        if var == 3:
                            k.op("pe", _mm(lg[:, 0:64], x1T[:, kc, ts_], wrh[:, kc, :], kc == 0, kc == 7), r=[x1T, wrh], w=[lg])
                            continue
                        k.op("pe", _mm(lg[:, 0:64], x1T[:, kc, ts_], wrh[:, kc, :], kc == 0, False), r=[x1T, wrh], w=[lg])
                        k.op("pe", _mm(lg[:, 0:64], x1Tl[:, kc, ts_], wrh[:, kc, :], False, False), r=[x1Tl, wrh], w=[lg])
                        k.op("pe", _mm(lg[:, 0:64], x1T[:, kc, ts_], wrl[:, kc, :], False, kc == 7), r=[x1T, wrl], w=[lg])
                    if self.fstop < 1.6:
                        continue
                    Lg = rt[:, 0:36]
                    gm, gs, gv = rt[:, 36:37], rt[:, 37:38], rt[:, 38:39]
                    ge, oh = rt[:, 40:44], rt[:, 44:48]
                    es_, e2, ex = rt[:, 48:56], rt[:, 56:64], rt[:, 64:72]
                    m1, m2, den = rt[:, 72:73], rt[:, 73:74], rt[:, 74:75]
                    sel2, wi = rt[:, 80:88], rt[:, 88:96]
                    R = [rt]
                    k.op("dve", _tt(Lg, lg[:, 0:36], brt[:, :], ALU.add), r=[lg, brt], w=R)
                    k.op("dve", lambda e: e.tensor_reduce(out=gm, in_=rt[:, 0:4], axis=AX.X, op=ALU.max), r=R, w=R)
                    k.op("dve", _ts(ge, rt[:, 0:4], gm, None, ALU.subtract), r=R, w=R)
                    k.op("act", _act(ge, ge, AF.Exp), r=R, w=R)
                    k.op("dve", lambda e: e.tensor_reduce(out=gs, in_=ge, axis=AX.X, op=ALU.add), r=R, w=R)
                    k.op("dve", lambda e: e.reciprocal(out=gv, in_=gs), r=R, w=R)
                    k.op("dve", _ts(oh, rt[:, 0:4], gm, None, ALU.is_ge), r=R, w=R)
                    k.op("dve", _ts(es_, rt[:, 4:12], oh[:, 0:1], None, ALU.mult), r=R, w=R)
                    for g in range(1, 4):
                        k.op("dve", _stt(es_, rt[:, 4 + 8 * g:12 + 8 * g], oh[:, g:g + 1], es_, ALU.mult, ALU.add), r=R, w=R)
                    k.op("dve", lambda e: e.tensor_reduce(out=m1, in_=es_, axis=AX.X, op=ALU.max), r=R, w=R)
                    k.op("dve", _ts(e2, es_, m1, -1e30, ALU.is_ge, ALU.mult), r=R, w=R)
                    k.op("dve", _tt(e2, e2, es_, ALU.add), r=R, w=R)
                    k.op("dve", lambda e: e.tensor_reduce(out=m2, in_=e2, axis=AX.X, op=ALU.max), r=R, w=R)
                    k.op("dve", _ts(sel2, es_, m2, None, ALU.is_ge), r=R, w=R)
                    k.op("dve", _ts(ex, es_, m1, None, ALU.subtract), r=R, w=R)
                    k.op("act", _act(ex, ex, AF.Exp), r=R, w=R)
                    k.op("dve", _tt(ex, ex, sel2, ALU.mult), r=R, w=R)
                    k.op("dve", lambda e: e.tensor_reduce(out=den, in_=ex, axis=AX.X, op=ALU.add), r=R, w=R)
                    k.op("dve", lambda e: e.reciprocal(out=den, in_=den), r=R, w=R)
                    k.op("dve", _tt(den, den, gv, ALU.mult), r=R, w=R)
                    k.op("dve", _ts(wi, ex, den, None, ALU.mult), r=R, w=R)
                    for g in range(4):
                        k.op("dve", _ts(comb[:, 8 * g:8 * g + 8], wi, oh[:, g:g + 1], None, ALU.mult), r=R, w=[comb])
                    if self.fstop < 1.8:
                        continue
                    k.op("dve", _cp(combb[:], comb[:]), r=[comb], w=[combb])
                    tpc = tpR.next()
                    k.op("pe", _tr(tpc[0:32, 0:128], combb[:, :], idb[:]), r=[combb, idb], w=[tpc])
                    k.op("act", lambda e: e.copy(out=combT[:, t * 128:(t + 1) * 128], in_=tpc[0:32, 0:128]), r=[tpc], w=[combT])
                if self.fstop < 3:
                    continue
                for g in range(4):
                    for e_ in range(8):
                        E = g * 8 + e_
                        wg, wgs = wgR.next()
                        wu, wus = wuR.next()
                        wd = wdS[e_]
                        k.dma("sp", wg[:], self.wg16[l].t[E].rearrange("(kc p) f -> p kc f", p=128), wgs, r=[self.wg16[l]], w=[wg])
                        k.dma("sp", wu[:], self.wu16[l].t[E].rearrange("(kc p) f -> p kc f", p=128), wus, r=[self.wu16[l]], w=[wu])
                        k.dma("sp", wd[:], self.wd16[l].t[E].rearrange("(fc p) d -> p fc d", p=128), wdsem[e_], r=[self.wd16[l]], w=[wd])
                        pbc = psR.next()
                        k.op("pe", _mm(pbc[:, :], sel[:, E, :], combT[:, :], True, True), r=[sel, combT], w=[pbc])
                        cb = cbR.next()
                        k.op("act", lambda e: e.copy(out=cb[:], in_=pbc[:, :]), r=[pbc], w=[cb])
                        for fc in range(2):
                            pg = psR.next()
                            pu = psR.next()
                            for kc in range(8):
                                k.op("pe", _mm(pg[:, :], wg[:, kc, fc * 128:(fc + 1) * 128], x1T[:, kc, :], kc == 0, kc == 7),
                                     r=[wg, x1T], w=[pg])
                            for kc in range(8):
                                k.op("pe", _mm(pu[:, :], wu[:, kc, fc * 128:(fc + 1) * 128], x1T[:, kc, :], kc == 0, kc == 7),
                                     r=[wu, x1T], w=[pu])
                            sgl = sgR.next()
                            k.op("act", _act(sgl[:], pg[:, :], AF.Silu), r=[pg], w=[sgl])
                            t1 = t1R.next()
                            k.op("dve", _tt(t1[:], pu[:, :], sgl[:], ALU.mult), r=[pu, sgl], w=[t1])
                            k.op("pool", _tt(hT[e_][fc][:], t1[:], cb[:], ALU.mult), r=[t1, cb], w=[hT[e_][fc]])
                    for t in range(4):
                        for dh in range(2):
                            py = psR.next()
                            first = True
                            for e_ in range(8):
                                for fc in range(2):
                                    k.op("pe", _mm(py[:, :], hT[e_][fc][:, t * 128:(t + 1) * 128], wdS[e_][:, fc, dh * 512:(dh + 1) * 512],
                                                   first, (e_ == 7 and fc == 1)), r=[hT[e_][fc], wdS[e_]], w=[py])
                                    first = False
                            ysl = yacc[:, t, dh * 512:(dh + 1) * 512]
                            if g == 0:
                                k.op("act", lambda e: e.copy(out=ysl, in_=py[:, :]), r=[py], w=[yacc])
                            else:
                                k.op("dve", _tt(ysl, ysl, py[:, :], ALU.add), r=[py, yacc], w=[yacc])
                if self.fstop < 4:
                    continue
                for t in range(4):
                    pT = pTR.next()
                    tpb = tpR.next()
                    for kc in range(2):
                        k.op("pe", _tr(tpb[:, kc * 128:(kc + 1) * 128], pt16[:, t, kc * 128:(kc + 1) * 128], idb[:]), r=[pt16, idb], w=[tpb])
                    k.op("act", lambda e: e.copy(out=pT[:].rearrange("p a b -> p (a b)"), in_=tpb[:, 0:256]), r=[tpb], w=[pT])
                    sig = sigR.next()
                    r_ = rR.next()
                    for half in range(2):
                        hs = slice(half * 512, (half + 1) * 512)
                        pp = psR.next()
                        for kc in range(2):
                            k.op("pe", _mm(pp[:, :], pT[:, kc, :], wple[:, kc, hs], kc == 0, kc == 1), r=[pT, wple], w=[pp])
                        pgt = psR.next()
                        for kc in range(8):
                            k.op("pe", _mm(pgt[:, :], x1T[:, kc, t * 128:(t + 1) * 128], wpg[:, kc, hs], kc == 0, kc == 7),
                                 r=[x1T, wpg], w=[pgt])
                        k.op("act", _act(sig[:, hs], pgt[:, :], AF.Sigmoid), r=[pgt], w=[sig])
                        k.op("dve", _tt(sig[:, hs], sig[:, hs], pp[:, :], ALU.mult), r=[sig, pp], w=[sig])
                    k.op("pool", _tt(r_[:], sig[:], yacc[:, t, :], ALU.add), r=[sig, yacc], w=[r_])
                    k.op("dve", _stt(r_[:], x1s[:, t, :], float(ALPHA), r_[:], ALU.mult, ALU.add), r=[x1s, r_], w=[r_])
                    ot, osem = oR.next()
                    self.layernorm(r_, g2, b2, ot[:, :], ot, st6, mv)
                    n = sg_i * 4 + t
                    k.dma("sp", dst.t[n * 128:(n + 1) * 128, :], ot[:], osem, r=[ot], w=[dst])
            for s_ in sems:
                k.put_dsem(s_)
            k.barrier()

    def do_gather(self):
        k = self.k
        e = k.eng["pool"]
        dsem = k.new_sem("ccsem")
        deps = k._deps([self.xown], [self.xall2], e, True)
        for idx, val in deps.items():
            e.wait(k.sems[idx], val)
        ins = self.nc.gpsimd.collective_compute("AllGather", ALU.bypass, replica_groups=[[0, 1, 2, 3], [4, 5, 6, 7]],
                                                ins=[self.xown.t[:, :]], outs=[self.xall2.t[:, :]])
        dsem.n += 16
        ins.then_inc(dsem.h, 16)
        self.xown.r[dsem.idx] = dsem.n
        self.xall2.w[dsem.idx] = dsem.n


_PROGS = {}


def _get_prog(S, layers):
    key = (S, tuple(layers))
    if key not in _PROGS:
        pr = Prog(S, list(layers), len(layers) > 1)
        pr.build()
        _PROGS[key] = pr
    return _PROGS[key]


def _own_idx(S, r):
    T = S // 4
    idx = np.concatenate([np.arange((4 * j + r) * 512, (4 * j + r + 1) * 512) for j in range(T // 512)])
    return idx


def _masks(r):
    ki = np.arange(512)
    qi = np.arange(512)
    cmT = np.zeros((128, 16, 512), np.float32)
    cmQ = np.zeros((128, 4, 512), np.float32)
    cmS = np.zeros((128, 8), np.float32)
    for sb in range(4):
        if sb < r:
            valid = np.ones((512, 512), bool)
        elif sb == r:
            valid = ki[:, None] <= qi[None, :]
        else:
            valid = np.zeros((512, 512), bool)
        for kb in range(4):
            cmT[:, sb * 4 + kb, :] = valid[kb * 128:(kb + 1) * 128, :]
        if sb == r:
            for qs in range(4):
                cmQ[:, qs, :] = np.where(valid[:, qs * 128:(qs + 1) * 128].T, 0.0, -1e30)
            cmS[:, 4 + sb] = 1.0
        elif sb > r:
            cmS[:, sb] = -1e30
    return cmT.reshape(128, -1), cmQ.reshape(128, -1), cmS


LAUNCH_PLAN = [[0], [1]]


def kernel(x, p, positions, w_in, diff_lambda, diff_subln_g, w_branch_diff, w_branch_dsa, w_out, ln1_g, ln1_b,
           w_route_group, b_route_group, w_route_expert, b_route_expert, w_exp_gate, w_exp_up, w_exp_down,
           w_ple, w_ple_gate, ln2_g, ln2_b):
    f = lambda a: np.ascontiguousarray(np.asarray(a, dtype=np.float32))
    x = f(x)
    p = f(p)
    positions = np.ascontiguousarray(np.asarray(positions, dtype=np.int32))
    B, S, _ = x.shape
    L = DEPTH
    T = S // 4
    shared = {
        "w_in": f(w_in), "diff_lambda": f(diff_lambda).reshape(L, 256), "diff_subln_g": f(diff_subln_g),
        "w_branch_diff": f(w_branch_diff), "w_branch_dsa": f(w_branch_dsa), "w_out": f(w_out),
        "ln1_g": f(ln1_g), "ln1_b": f(ln1_b), "w_route_group": f(w_route_group), "b_route_group": f(b_route_group),
        "w_route_expert": f(w_route_expert), "b_route_expert": f(b_route_expert),
        "w_exp_gate": f(w_exp_gate).reshape(L, 32, D, 256), "w_exp_up": f(w_exp_up).reshape(L, 32, D, 256),
        "w_exp_down": f(w_exp_down).reshape(L, 32, 256, D), "w_ple": f(w_ple), "w_ple_gate": f(w_ple_gate),
        "ln2_g": f(ln2_g), "ln2_b": f(ln2_b),
    }
    own = [_own_idx(S, r) for r in range(4)]
    masks = [_masks(r) for r in range(4)]
    cur = x
    for layers in LAUNCH_PLAN:
        pr = _get_prog(S, layers)
        in_maps = []
        for c in range(8):
            b, r = divmod(c, 4)
            m = dict(shared)
            m["xall"] = np.ascontiguousarray(np.concatenate([cur[b][own[rr]] for rr in range(4)], axis=0))
            m["xq"] = np.ascontiguousarray(cur[b][own[r]])
            m["p"] = np.ascontiguousarray(p[:, b][:, own[r]])
            m["posall"] = np.ascontiguousarray(positions[b].reshape(S // 128, 128).T)
            m["posq"] = np.ascontiguousarray(positions[b][own[r]].reshape(T // 128, 128).T)
            m["cmT"], m["cmQ"], m["cmS"] = masks[r]
            in_maps.append(m)
        res = run_bass_kernel_spmd(pr.nc, in_maps, core_ids=list(range(8)))
        nxt = np.empty_like(x)
        for c in range(8):
            b, r = divmod(c, 4)
            nxt[b][own[r]] = res.results[c]["out"]
        cur = nxt
    return cur
```
